# Optimizing a Trainium2 kernel written in Bass

```python
import math
import jax, jax.numpy as jnp
from jax import lax
import numpy as np

D_MODEL = 1024
BATCH = 8
SEQ = 4096
DEPTH = 4

HEAD_DIM = 64
POOL_WINDOWS = (2, 4, 8, 16)
POOL_GROUP_DIM = 64
POOL_WIDTH = len(POOL_WINDOWS) * POOL_GROUP_DIM
DIL_CONFIGS = ((128, 1), (512, 4), (2048, 16))
DIL_HEADS = 4
N_DIL_GROUPS = len(DIL_CONFIGS)
DIL_GROUP_WIDTH = DIL_HEADS * HEAD_DIM
DIL_WIDTH = N_DIL_GROUPS * DIL_GROUP_WIDTH
BAND_BLOCK = 128
EVEN_IN = POOL_WIDTH + 3 * DIL_WIDTH
EVEN_OUT = POOL_WIDTH + DIL_GROUP_WIDTH
FOX_HEADS = D_MODEL // HEAD_DIM
FOX_WIDTH = FOX_HEADS * HEAD_DIM
ODD_IN = 3 * FOX_WIDTH + FOX_HEADS
Q_BLOCK = 128
ROPE_THETA = 500000.0
ROPE_DIM = HEAD_DIM // 4
D_FF = 2816
N_EXPERTS = 8
TOP_K = 2
D_FF_EXPERT = 2816
DEEPNORM_ALPHA = (2 * DEPTH) ** 0.25
DEEPNORM_BETA = (8 * DEPTH) ** -0.25
LN_EPS = 1e-5
NEG = -1e30
N_EVEN = (DEPTH + 1) // 2
N_ODD = DEPTH // 2

kernel_name = "hybrid_pool_dilated_fox_moe_trunk"


def layer_norm(x, g, b):
    xf = x.astype(jnp.float32)
    mu = jnp.mean(xf, axis=-1, keepdims=True)
    var = jnp.mean(jnp.square(xf - mu), axis=-1, keepdims=True)
    return ((xf - mu) * lax.rsqrt(var + LN_EPS) * g.astype(jnp.float32) + b.astype(jnp.float32)).astype(x.dtype)


def rope_tables(seq):
    pos = jnp.arange(seq, dtype=jnp.float32)
    inv = ROPE_THETA ** (-jnp.arange(0, ROPE_DIM, 2, dtype=jnp.float32) / ROPE_DIM)
    ang = pos[:, None] * inv[None, :]
    return jnp.cos(ang), jnp.sin(ang)


def partial_rope(x, cos, sin):
    xr = x[..., :ROPE_DIM].astype(jnp.float32)
    x1, x2 = xr[..., :ROPE_DIM // 2], xr[..., ROPE_DIM // 2:]
    c = cos[None, :, None, :]
    s = sin[None, :, None, :]
    rot = jnp.concatenate([x1 * c - x2 * s, x1 * s + x2 * c], axis=-1).astype(x.dtype)
    return jnp.concatenate([rot, x[..., ROPE_DIM:]], axis=-1)


def multiscale_pool(u, w_pool, pool_scale):
    B, S, _ = u.shape
    ug = u.reshape(B, S, len(POOL_WINDOWS), POOL_GROUP_DIM).astype(jnp.float32)
    cs = jnp.pad(jnp.cumsum(ug, axis=1), ((0, 0), (1, 0), (0, 0), (0, 0)))
    t = jnp.arange(S, dtype=jnp.float32)
    pooled = []
    for g, w in enumerate(POOL_WINDOWS):
        hi = cs[:, 1:, g]
        lo = jnp.pad(cs[:, :S + 1 - w, g], ((0, 0), (w - 1, 0), (0, 0)))
        count = jnp.minimum(t + 1.0, float(w))[None, :, None]
        pooled.append((hi - lo) / count - ug[:, :, g])
    pooled = jnp.stack(pooled, axis=2)
    out = jnp.einsum('bsgc,gcd->bsgd', pooled, w_pool.astype(jnp.float32))
    return (out.reshape(B, S, POOL_WIDTH) * pool_scale.astype(jnp.float32)).astype(u.dtype)


def banded_causal_attention(q, k, v, span):
    assert span <= BAND_BLOCK
    N, L, H, Dh = q.shape
    Qb = BAND_BLOCK
    nb = -(-L // Qb)
    Lp = nb * Qb
    qb = jnp.pad(q, ((0, 0), (0, Lp - L), (0, 0), (0, 0))).reshape(N, nb, Qb, H, Dh)
    padk = ((0, 0), (Qb, Lp - L), (0, 0), (0, 0))
    kp = jnp.pad(k, padk).reshape(N, nb + 1, Qb, H, Dh)
    vp = jnp.pad(v, padk).reshape(N, nb + 1, Qb, H, Dh)
    kb = jnp.concatenate([kp[:, :-1], kp[:, 1:]], axis=2)
    vb = jnp.concatenate([vp[:, :-1], vp[:, 1:]], axis=2)
    s = jnp.einsum('nbqhd,nbkhd->nbhqk', qb, kb, preferred_element_type=jnp.float32) * (HEAD_DIM ** -0.5)
    qi = jnp.arange(Qb)[:, None]
    kj = jnp.arange(2 * Qb)[None, :]
    dist = qi + Qb - kj
    kpos = jnp.arange(nb)[:, None, None] * Qb - Qb + kj[None]
    mask = (dist >= 0)[None] & (dist <= span)[None] & (kpos >= 0)
    s = jnp.where(mask[None, :, None], s, NEG)
    m = jnp.max(s, axis=-1, keepdims=True)
    p = jnp.exp(s - m)
    den = jnp.sum(p, axis=-1)
    o = jnp.einsum('nbhqk,nbkhd->nbqhd', p, vb.astype(jnp.float32))
    o = o / jnp.transpose(den, (0, 1, 3, 2))[..., None]
    lse = jnp.transpose(m[..., 0] + jnp.log(den), (0, 1, 3, 2))
    return o.reshape(N, Lp, H, Dh)[:, :L], lse.reshape(N, Lp, H)[:, :L]


def dilated_attention(q, k, v, window, dilation):
    B, S, H, Dh = q.shape
    L = S // dilation

    def to_strided(a):
        return a.reshape(B, L, dilation, H, Dh).transpose(0, 2, 1, 3, 4).reshape(B * dilation, L, H, Dh)

    o, lse = banded_causal_attention(to_strided(q), to_strided(k), to_strided(v), window // dilation)
    o = o.reshape(B, dilation, L, H, Dh).transpose(0, 2, 1, 3, 4).reshape(B, S, H, Dh)
    lse = lse.reshape(B, dilation, L, H).transpose(0, 2, 1, 3).reshape(B, S, H)
    return o, lse


def even_mixer(h, w_in, w_pool, pool_scale, w_out, cos, sin):
    B, S, _ = h.shape
    z = h @ w_in
    a_out = multiscale_pool(z[..., :POOL_WIDTH], w_pool, pool_scale)
    qkv = z[..., POOL_WIDTH:].reshape(B, S, 3, N_DIL_GROUPS, DIL_HEADS, HEAD_DIM)
    outs, lses = [], []
    for g, (window, dilation) in enumerate(DIL_CONFIGS):
        q = partial_rope(qkv[:, :, 0, g], cos, sin)
        k = partial_rope(qkv[:, :, 1, g], cos, sin)
        o, lse = dilated_attention(q, k, qkv[:, :, 2, g], window, dilation)
        outs.append(o)
        lses.append(lse)
    wts = jax.nn.softmax(jnp.stack(lses, axis=0), axis=0)
    b_out = jnp.einsum('gbsh,gbshd->bshd', wts, jnp.stack(outs, axis=0))
    mixed = jnp.concatenate([a_out, b_out.reshape(B, S, DIL_GROUP_WIDTH).astype(h.dtype)], axis=-1)
    return mixed @ w_out


def forgetting_attention(q, k, v, f_logit):
    B, S, H, Dh = q.shape
    logf = jax.nn.log_sigmoid(f_logit.astype(jnp.float32))
    F = jnp.cumsum(logf, axis=1)
    Fk = jnp.transpose(F, (0, 2, 1))
    nb = S // Q_BLOCK
    qb = q.reshape(B, nb, Q_BLOCK, H, Dh).transpose(1, 0, 2, 3, 4)
    Fq = Fk.reshape(B, H, nb, Q_BLOCK).transpose(2, 0, 1, 3)
    kpos = jnp.arange(S)

    def block(args):
        qi, Fi, i = args
        s = jnp.einsum('bqhd,bkhd->bhqk', qi, k, preferred_element_type=jnp.float32) * (HEAD_DIM ** -0.5)
        s = s + (Fi[..., None] - Fk[:, :, None, :])
        qpos = i * Q_BLOCK + jnp.arange(Q_BLOCK)
        s = jnp.where(kpos[None, :] <= qpos[:, None], s, NEG)
        p = jax.nn.softmax(s, axis=-1)
        return jnp.einsum('bhqk,bkhd->bqhd', p.astype(v.dtype), v)

    out = lax.map(block, (qb, Fq, jnp.arange(nb)))
    return out.transpose(1, 0, 2, 3, 4).reshape(B, S, H, Dh)


def odd_mixer(h, w_in, b_forget, w_out):
    B, S, _ = h.shape
    z = h @ w_in
    qkv = z[..., :3 * FOX_WIDTH].reshape(B, S, 3, FOX_HEADS, HEAD_DIM)
    f_logit = z[..., 3 * FOX_WIDTH:] + b_forget
    o = forgetting_attention(qkv[:, :, 0], qkv[:, :, 1], qkv[:, :, 2], f_logit)
    return o.reshape(B, S, FOX_WIDTH) @ w_out


def swiglu(h, w_gu, w_down):
    g, u = jnp.split(h @ w_gu, 2, axis=-1)
    return (jax.nn.silu(g) * u) @ w_down


def moe_swiglu(h, w_router, w_gu_e, w_down_e):
    logits = (h @ w_router).astype(jnp.float32)
    top_v, top_i = lax.top_k(logits, TOP_K)
    gates = jax.nn.softmax(top_v, axis=-1)
    combine = jnp.sum(jax.nn.one_hot(top_i, N_EXPERTS, dtype=jnp.float32) * gates[..., None], axis=-2)
    out = jnp.zeros_like(h)
    for e in range(N_EXPERTS):
        out = out + combine[..., e:e + 1].astype(h.dtype) * swiglu(h, w_gu_e[e], w_down_e[e])
    return out


def setup_inputs(seed: int = 0) -> dict:
    key = jax.random.key(seed)
    ks = jax.random.split(key, 20)
    f32 = jnp.float32
    D = D_MODEL

    def nrm(k, shape, fan_in, gain=1.0):
        return jax.random.normal(k, shape, f32) * (gain * fan_in ** -0.5)

    return {
        "x": jax.random.normal(ks[0], (BATCH, SEQ, D), f32),
        "c": jax.random.normal(ks[1], (BATCH, D), f32),
        "w_ada": nrm(ks[2], (DEPTH, D, 6 * D), D),
        "b_ada": 0.02 * jax.random.normal(ks[3], (DEPTH, 6 * D), f32),
        "ln_g": 1.0 + 0.05 * jax.random.normal(ks[4], (DEPTH, 2, D), f32),
        "ln_b": 0.02 * jax.random.normal(ks[5], (DEPTH, 2, D), f32),
        "w_in_even": nrm(ks[6], (N_EVEN, D, EVEN_IN), D),
        "w_pool": nrm(ks[7], (N_EVEN, len(POOL_WINDOWS), POOL_GROUP_DIM, POOL_GROUP_DIM), POOL_GROUP_DIM),
        "pool_scale": 1.0 + 0.1 * jax.random.normal(ks[8], (N_EVEN, POOL_WIDTH), f32),
        "w_out_even": nrm(ks[9], (N_EVEN, EVEN_OUT, D), EVEN_OUT, DEEPNORM_BETA),
        "w_ffn_gu": nrm(ks[10], (N_EVEN, D, 2 * D_FF), D),
        "w_ffn_down": nrm(ks[11], (N_EVEN, D_FF, D), D_FF, DEEPNORM_BETA),
        "w_in_odd": nrm(ks[12], (N_ODD, D, ODD_IN), D),
        "b_forget": jax.random.uniform(ks[13], (N_ODD, FOX_HEADS), f32, 1.0, 4.0),
        "w_out_odd": nrm(ks[14], (N_ODD, FOX_WIDTH, D), FOX_WIDTH, DEEPNORM_BETA),
        "w_router": nrm(ks[15], (N_ODD, D, N_EXPERTS), D),
        "w_exp_gu": nrm(ks[16], (N_ODD, N_EXPERTS, D, 2 * D_FF_EXPERT), D),
        "w_exp_down": nrm(ks[17], (N_ODD, N_EXPERTS, D_FF_EXPERT, D), D_FF_EXPERT, DEEPNORM_BETA),
    }


def reference(x, c, w_ada, b_ada, ln_g, ln_b, w_in_even, w_pool, pool_scale, w_out_even,
              w_ffn_gu, w_ffn_down, w_in_odd, b_forget, w_out_odd, w_router, w_exp_gu, w_exp_down):
    cos, sin = rope_tables(x.shape[1])
    c_act = jax.nn.silu(c)
    for l in range(DEPTH):
        ada = (c_act @ w_ada[l] + b_ada[l])[:, None, :]
        sh1, sc1, g1, sh2, sc2, g2 = jnp.split(ada, 6, axis=-1)
        h = x * (1 + sc1) + sh1
        if l % 2 == 0:
            i = l // 2
            sub = even_mixer(h, w_in_even[i], w_pool[i], pool_scale[i], w_out_even[i], cos, sin)
        else:
            i = l // 2
            sub = odd_mixer(h, w_in_odd[i], b_forget[i], w_out_odd[i])
        x = layer_norm(DEEPNORM_ALPHA * x + g1 * sub, ln_g[l, 0], ln_b[l, 0])
        h = x * (1 + sc2) + sh2
        if l % 2 == 0:
            sub = swiglu(h, w_ffn_gu[l // 2], w_ffn_down[l // 2])
        else:
            sub = moe_swiglu(h, w_router[l // 2], w_exp_gu[l // 2], w_exp_down[l // 2])
        x = layer_norm(DEEPNORM_ALPHA * x + g2 * sub, ln_g[l, 1], ln_b[l, 1])
    return x
```

```python
from concourse.bass_utils import run_bass_kernel_spmd
from contextlib import ExitStack
import numpy as np
import concourse.bass as bass
import concourse.mybir as mybir

F32 = mybir.dt.float32
BF16 = mybir.dt.bfloat16
ALU = mybir.AluOpType
AF = mybir.ActivationFunctionType

ENGS = ["pe", "act", "dve", "pool", "sp"]
BLOCKNAME = {"pe": "tensor", "act": "scalar", "dve": "vector", "pool": "gpsimd", "sp": "sync"}


class Buf:
    __slots__ = ("name", "w", "r", "dsem", "dcnt", "multi", "ws")

    def __init__(self, name, multi=False):
        self.name = name
        self.multi = multi
        self.ws = {}
        self.w = None
        self.r = {}
        self.dsem = None
        self.dcnt = 0


class Rec:
    __slots__ = ("eng", "fn", "waits", "signal", "sigidx", "dsem", "flushed", "self_sync")

    def __init__(self, eng, fn):
        self.eng = eng
        self.fn = fn
        self.waits = []
        self.signal = False
        self.sigidx = None
        self.dsem = None
        self.flushed = False
        self.self_sync = False


def _tkey(tok):
    if tok[0] == "e":
        return ("e", tok[1].eng)
    return ("d", id(tok[1]))


class Prog:
    def __init__(self, nc, stack, n_dsem=150):
        self.nc = nc
        self.stack = stack
        self.esem = {e: stack.enter_context(nc.semaphore("es_" + e)) for e in ENGS}
        self.free_dsems = [[stack.enter_context(nc.semaphore("ds%d" % i)), 0] for i in range(n_dsem)]
        self.all_dsem_bufs = []
        self.sigcount = {e: 0 for e in ENGS}
        self.waited = {e: {} for e in ENGS}
        self.ops = {e: [] for e in ENGS}
        self.barrier_toks = {e: [] for e in ENGS}
        self.nops = 0

    def _deps(self, rec, reads, writes, include_same_engine):
        deps = []
        for b in reads:
            if b.multi:
                deps.extend(b.ws.values())
            elif b.w is not None:
                deps.append(b.w)
        for b in writes:
            if (not b.multi) and b.w is not None:
                deps.append(b.w)
            deps.extend(b.r.values())
        if self.barrier_toks[rec.eng]:
            for t in self.barrier_toks[rec.eng]:
                if t[0] == "e" and t[1].eng == rec.eng and not include_same_engine:
                    continue
                rec.waits.append(t)
            self.barrier_toks[rec.eng] = []
        seen = set()
        for t in deps:
            if t[0] == "e":
                if t[1].flushed:
                    continue
                if t[1].eng == rec.eng and not include_same_engine:
                    continue
                if id(t[1]) in seen:
                    continue
                seen.add(id(t[1]))
                t[1].signal = True
            rec.waits.append(t)

    def _commit(self, tok, reads, writes):
        k = _tkey(tok)
        for b in reads:
            b.r[k] = tok
        for b in writes:
            if b.multi:
                if b.r:
                    b.ws = {}
                    b.r = {}
                b.ws[k] = tok
            else:
                b.w = tok
                b.r = {}

    def op(self, eng, fn, reads=(), writes=(), sync_same=False):
        rec = Rec(eng, fn)
        rec.self_sync = sync_same
        self._deps(rec, reads, writes, sync_same)
        tok = ("e", rec)
        self._commit(tok, reads, writes)
        self.ops[eng].append(rec)
        self.nops += 1
        return rec

    def dma(self, q, out, in_, sembuf, reads=(), writes=(), **kw):
        rec = Rec(q, lambda e: e.dma_start(out=out, in_=in_, **kw))
        self._deps(rec, reads, writes, True)
        if sembuf.dsem is None:
            sembuf.dsem, sembuf.dcnt = self.free_dsems.pop()
            self.all_dsem_bufs.append(sembuf)
        sembuf.dcnt += 1
        rec.dsem = sembuf.dsem
        tok = ("d", sembuf.dsem, 16 * sembuf.dcnt)
        self._commit(tok, reads, writes)
        self.ops[q].append(rec)
        self.nops += 1
        return rec

    def release_dsem(self, buf):
        pass

    def _resolve(self, tok):
        if tok[0] == "e":
            r = tok[1]
            return self.esem[r.eng], r.sigidx
        return tok[1], tok[2]

    def flush(self, barrier=True):
        nc = self.nc
        if barrier:
            toks = []
            for e in ENGS:
                if self.ops[e]:
                    last = self.ops[e][-1]
                    if last.dsem is None:
                        last.signal = True
                        toks.append(("e", last))
                    else:
                        for r in reversed(self.ops[e]):
                            if r.dsem is None:
                                r.signal = True
                                toks.append(("e", r))
                                break
            for b in self.all_dsem_bufs:
                toks.append(("d", b.dsem, 16 * b.dcnt))
        for e in ENGS:
            for rec in self.ops[e]:
                if rec.signal:
                    self.sigcount[e] += 1
                    rec.sigidx = self.sigcount[e]
        with nc.Block() as block:
            for e in ENGS:
                if not self.ops[e]:
                    continue

                def body(engh, e=e):
                    wd = self.waited[e]
                    for rec in self.ops[e]:
                        for tok in rec.waits:
                            sem, val = self._resolve(tok)
                            if sem is self.esem[e] and rec.dsem is None and not rec.self_sync:
                                continue
                            k = id(sem)
                            if wd.get(k, 0) < val:
                                engh.wait_ge(sem, val)
                                wd[k] = val
                        ins = rec.fn(engh)
                        if rec.dsem is not None:
                            ins.then_inc(rec.dsem, 16)
                        elif rec.signal:
                            ins.then_inc(self.esem[e], 1)

                getattr(block, BLOCKNAME[e])(body)
        assert barrier
        for e in ENGS:
            for rec in self.ops[e]:
                rec.flushed = True
        self.ops = {e: [] for e in ENGS}
        if barrier:
            for e in ENGS:
                self.barrier_toks[e] = list(toks)
            for b in self.all_dsem_bufs:
                self.free_dsems.insert(0, [b.dsem, b.dcnt])
                b.dsem = None
            self.all_dsem_bufs = []

    def finish(self):
        toks = self.barrier_toks["sp"]
        nc = self.nc
        with nc.Block() as block:
            def body(engh):
                for tok in toks:
                    sem, val = self._resolve(tok)
                    engh.wait_ge(sem, val)
            block.sync(body)
S = 4096
D = 1024
NT = 32
DFF = 2816
NFF = 22
ALPHA = (2 * 4) ** 0.25
LN_EPS = 1e-5


def host_consts():
    import ml_dtypes
    c = {}
    c["ident"] = np.eye(128, dtype=np.float32).astype(ml_dtypes.bfloat16)
    pos = np.arange(S, dtype=np.float32)
    inv = (500000.0 ** (-np.arange(0, 16, 2, dtype=np.float32) / 16)).astype(np.float32)
    ang = pos[None, :] * inv[:, None]
    cos = np.cos(ang).astype(np.float32)
    sin = np.sin(ang).astype(np.float32)
    C = np.ones((128, S), np.float32)
    Sn = np.zeros((128, S), np.float32)
    for p in range(128):
        dd = p % 64
        if dd < 16:
            C[p] = cos[dd % 8]
            Sn[p] = sin[dd % 8]
    c["ropeC"] = C
    c["ropeS"] = Sn
    RT = np.zeros((128, 128), np.float32)
    for hb in (0, 64):
        for i in range(8):
            RT[hb + i + 8, hb + i] = -1.0
            RT[hb + i, hb + i + 8] = 1.0
    c["ropeRT"] = RT.astype(ml_dtypes.bfloat16)
    i = np.arange(128)[:, None]
    j = np.arange(128)[None, :]
    NEGM = -30000.0
    c["maskcur"] = np.where(j >= i, 0.0, NEGM).astype(np.float32).astype(ml_dtypes.bfloat16)
    c["maskprev"] = np.where(j <= i, 0.0, NEGM).astype(np.float32).astype(ml_dtypes.bfloat16)
    invw = np.zeros((128, 2), np.float32)
    invc = np.zeros((128, 2, 16), np.float32)
    for b in range(2):
        for p in range(128):
            w = 2 ** (2 * b + p // 64 + 1)
            invw[p, b] = 1.0 / w
            invc[p, b, :] = 1.0 / np.minimum(np.arange(16) + 1.0, float(w))
    c["invw"] = invw
    c["invc"] = invc
    return c


CONST_SPECS = [("ident", [128, 128], "bf16"), ("ropeC", [128, S], "f32"), ("ropeS", [128, S], "f32"),
               ("ropeRT", [128, 128], "bf16"), ("maskcur", [128, 128], "bf16"), ("maskprev", [128, 128], "bf16"),
               ("invw", [128, 2], "f32"), ("invc", [128, 2, 16], "f32")]

IN_SPECS = [("x", [S, D]), ("c", [D]), ("w_ada", [4, D, 6 * D]), ("b_ada", [4, 6 * D]), ("ln_g", [4, 2, D]),
            ("ln_b", [4, 2, D]), ("w_in_even", [2, D, 2560]), ("w_pool", [2, 4, 64, 64]), ("pool_scale", [2, 256]),
            ("w_out_even", [2, 512, D]), ("w_ffn_gu", [2, D, 2 * DFF]), ("w_ffn_down", [2, DFF, D]),
            ("w_in_odd", [2, D, 3088]), ("b_forget", [2, 16]), ("w_out_odd", [2, D, D]), ("w_router", [2, D, 8]),
            ("w_exp_gu", [2, 8, D, 2 * DFF]), ("w_exp_down", [2, 8, DFF, D])]


class K:
    pass


def needed_inputs(s0, s1):
    need = {"x", "c", "w_ada", "b_ada", "ln_g", "ln_b"}
    for s in range(s0, s1):
        l, j = divmod(s, 2)
        if l % 2 == 0 and j == 0:
            need |= {"w_in_even", "w_pool", "pool_scale", "w_out_even"}
        elif l % 2 == 0:
            need |= {"w_ffn_gu", "w_ffn_down"}
        elif j == 0:
            need |= {"w_in_odd", "b_forget", "w_out_odd"}
            if s + 1 < s1:
                need |= {"w_router"}
        else:
            need |= {"w_exp_gu", "w_exp_down"}
    return need


def build(s0=0, s1=8, debug=()):
    nc = bass.Bass("TRN2", target_bir_lowering=False)
    k = K()
    k.nc = nc
    I = {}
    need = needed_inputs(s0, s1)
    for name, shape in IN_SPECS:
        if name in need:
            I[name] = nc.dram_tensor(name, shape, F32, kind="ExternalInput").ap()
    for name, shape, dt in CONST_SPECS:
        I[name] = nc.dram_tensor(name, shape, F32 if dt == "f32" else BF16, kind="ExternalInput").ap()
    k.I = I
    dbg = set(debug)

    def scratch(name, shape, dt):
        kind = "ExternalOutput" if name in dbg else "Internal"
        return nc.dram_tensor(name, shape, dt, kind=kind).ap()

    k.out = nc.dram_tensor("out", [S, D], F32, kind="ExternalOutput").ap()
    k.ada_d = scratch("ada_d", [4, 6 * D], F32)
    k.hT_d = scratch("hT_d", [D, S], BF16)
    k.xres = scratch("xres", [S, D], F32)
    k.zq_d = scratch("zq_d", [3, 256, S], BF16)
    k.zv_d = scratch("zv_d", [3, S, 4 * 65], BF16)
    k.mixT_d = scratch("mixT_d", [D, S], BF16)
    k.qa_d = scratch("qa_d", [16, 70, S], BF16)
    k.ka_d = scratch("ka_d", [16, 70, S], BF16)
    k.va_d = scratch("va_d", [S, 16 * 65], BF16)
    k.comb_d = scratch("comb_d", [S, 8], F32)
    k.sub_d = scratch("sub_d", [S, D], F32)

    with ExitStack() as gst:
        p = Prog(nc, gst, n_dsem=90)
        k.p = p
        k.ps = [gst.enter_context(nc.psum_tensor("psb%d" % i, [128, 512], F32)) for i in range(7)]
        k.Bps = [Buf("ps%d" % i) for i in range(7)]
        k.pstb = gst.enter_context(nc.psum_tensor("pstb", [128, 1024], BF16))
        k.Bpstb = Buf("pstb")
        k.ident = gst.enter_context(nc.sbuf_tensor("sb_ident", [128, 128], BF16))
        k.Bident = Buf("ident")
        p.dma("sp", k.ident[:, :], I["ident"][:, :], k.Bident, writes=[k.Bident])
        k.B_ada = Buf("ada_d", multi=True)
        k.B_hT = [Buf("hT%d" % i) for i in range(8)]
        k.B_x = [Buf("xres%d" % i) for i in range(NT)]
        k.B_mix = [Buf("mix%d" % i, multi=True) for i in range(8)]
        k.B_sub = [Buf("sub%d" % i) for i in range(NT)]
        k.B_comb = Buf("comb", multi=True)
        k.B_qk = Buf("qk_d", multi=True)
        k.B_va = Buf("va_d", multi=True)
        k.B_zq = Buf("zq_d", multi=True)
        k.B_zv = Buf("zv_d", multi=True)

        k.s0, k.s1 = s0, s1
        phase_prologue(k)
        phase_h0(k)
        for s in range(s0, s1):
            l, j = divmod(s, 2)
            last = (s == s1 - 1)
            if j == 0 and l % 2 == 0:
                even_mixer(k, l, s, last)
            elif j == 0:
                odd_mixer(k, l, s, last)
            elif l % 2 == 0:
                ffn_layer(k, l, s, last, moe=False)
            else:
                ffn_layer(k, l, s, last, moe=True)
        p.finish()
    return nc


_UID = [0]


def sbt(k, st, name, shape, dt):
    _UID[0] += 1
    return st.enter_context(k.nc.sbuf_tensor("sb%d_%s" % (_UID[0], name), shape, dt))


def mm(p, out, lhsT, rhs, start, stop, reads, writes):
    return p.op("pe", lambda e: e.matmul(out, lhsT=lhsT, rhs=rhs, start=start, stop=stop), reads, writes)


def trp(p, out, in_, ident, reads, writes):
    return p.op("pe", lambda e: e.transpose(out, in_, ident), reads, writes)


def actf(p, out, in_, func, reads, writes, bias=None, scale=None):
    kw = {}
    if bias is not None:
        kw["bias"] = bias
    if scale is not None:
        kw["scale"] = scale
    return p.op("act", lambda e: e.activation(out=out, in_=in_, func=func, **kw), reads, writes)


def tt(p, eng, out, in0, in1, op, reads, writes, ss=False):
    return p.op(eng, lambda e: e.tensor_tensor(out=out, in0=in0, in1=in1, op=op), reads, writes, sync_same=ss)


def tsc(p, eng, out, in0, s1, op0, reads, writes, s2=None, op1=None, ss=False):
    if op1 is None:
        return p.op(eng, lambda e: e.tensor_scalar(out=out, in0=in0, scalar1=s1, scalar2=None, op0=op0), reads, writes,
                    sync_same=ss)
    return p.op(eng, lambda e: e.tensor_scalar(out=out, in0=in0, scalar1=s1, scalar2=s2, op0=op0, op1=op1),
                reads, writes, sync_same=ss)


def stt(p, eng, out, in0, scalar, in1, op0, op1, reads, writes, ss=False):
    eng = "dve"
    return p.op(eng, lambda e: e.scalar_tensor_tensor(out=out, in0=in0, scalar=scalar, in1=in1, op0=op0, op1=op1),
                reads, writes, sync_same=ss)


def cpy(p, eng, out, in_, reads, writes, ss=False):
    if eng == "act":
        return p.op("act", lambda e: e.copy(out=out, in_=in_), reads, writes, sync_same=ss)
    return p.op(eng, lambda e: e.tensor_copy(out=out, in_=in_), reads, writes, sync_same=ss)


def mset(p, eng, ap, val, writes):
    return p.op(eng, lambda e: e.memset(ap, val), (), writes)


def ada_slice(k, l, idx):
    return k.ada_d[l, idx * D:(idx + 1) * D]


def phase_prologue(k):
    p, nc, I = k.p, k.nc, k.I
    with ExitStack() as st:
        ccol = sbt(k, st, "ccol", [128, 8], F32)
        cact = sbt(k, st, "cact", [128, 8], F32)
        wst = [sbt(k, st, "wst%d" % i, [128, 8, 512], F32) for i in range(2)]
        brow = sbt(k, st, "brow", [1, 6 * D], F32)
        arow = sbt(k, st, "arow", [1, 6 * D], F32)
        Bc, Bca, Bbr, Bar = Buf("ccol"), Buf("cact"), Buf("brow"), Buf("arow")
        Bw = [Buf("wst0"), Buf("wst1")]
        for kk in range(8):
            p.dma("sp", ccol[:, kk:kk + 1], I["c"][kk * 128:(kk + 1) * 128].rearrange("(p o) -> p o", o=1), Bc,
                  writes=[Bc])
        actf(p, cact[:, :], ccol[:, :], AF.Silu, [Bc], [Bca])
        it = 0
        for l in range(4):
            p.dma("sp", brow[0:1, :], I["b_ada"][l:l + 1, :], Bbr, writes=[Bbr])
            for n in range(12):
                sl = it % 2
                bank = it % 2
                it += 1
                p.dma("sp", wst[sl][:, :, :],
                      I["w_ada"][l, :, n * 512:(n + 1) * 512].rearrange("(k p) n -> p k n", p=128),
                      Bw[sl], writes=[Bw[sl]])
                for kk in range(8):
                    mm(p, k.ps[bank][0:1, :], cact[:, kk:kk + 1], wst[sl][:, kk, :], kk == 0, kk == 7,
                       [Bca, Bw[sl]], [k.Bps[bank]])
                tt(p, "dve", arow[0:1, n * 512:(n + 1) * 512], k.ps[bank][0:1, :], brow[0:1, n * 512:(n + 1) * 512],
                   ALU.add, [k.Bps[bank], Bbr], [Bar])
            p.dma("sp", k.ada_d[l:l + 1, :], arow[0:1, :], Bar, reads=[Bar], writes=[k.B_ada])
        p.flush()


class HT:
    def __init__(self, k, st, tag):
        self.hst = [sbt(k, st, "hst%s%d" % (tag, i), [128, 8, 512], BF16) for i in range(2)]
        self.B = [Buf("hst0"), Buf("hst1")]
        self.pst = k.pstb
        self.Bp = k.Bpstb


def emit_ht(k, H, hb, Bhb, i):
    p = k.p
    c, j = divmod(i, 4)
    sl = c % 2
    for kk in range(8):
        trp(p, H.pst[:, kk * 128:(kk + 1) * 128], hb[:, kk * 128:(kk + 1) * 128], k.ident[:, :],
            [Bhb, k.Bident], [H.Bp])
    cpy(p, "act", H.hst[sl][:, :, j * 128:(j + 1) * 128], H.pst[:, :].rearrange("p (k t) -> p k t", k=8),
        [H.Bp], [H.B[sl]])
    if j == 3:
        p.dma("sp", k.hT_d.rearrange("(k p) t -> p k t", p=128)[:, :, c * 512:(c + 1) * 512], H.hst[sl][:, :, :],
              H.B[sl], reads=[H.B[sl]], writes=[k.B_hT[c]])


def load_bc(k, p, dst, Bdst, src_row):
    p.dma("sp", dst[:, :], src_row.partition_broadcast(128), Bdst, reads=[k.B_ada], writes=[Bdst])


def phase_h0(k):
    p, nc, I = k.p, k.nc, k.I
    with ExitStack() as st:
        sc = sbt(k, st, "h0sc", [128, D], F32)
        sh = sbt(k, st, "h0sh", [128, D], F32)
        Bsc, Bsh = Buf("sc"), Buf("sh")
        xo = [sbt(k, st, "h0x%d" % i, [128, D], F32) for i in range(2)]
        Bxo = [Buf("xo0"), Buf("xo1")]
        hb = [sbt(k, st, "h0hb%d" % i, [128, D], BF16) for i in range(2)]
        Bhb = [Buf("hb0"), Buf("hb1")]
        H = HT(k, st, "a")
        l0, j0 = divmod(k.s0, 2)
        load_bc(k, p, sc, Bsc, ada_slice(k, l0, 3 * j0 + 1))
        load_bc(k, p, sh, Bsh, ada_slice(k, l0, 3 * j0 + 0))
        tsc(p, "dve", sc[:, :], sc[:, :], 1.0, ALU.add, [Bsc], [Bsc])
        for i in range(NT):
            sl = i % 2
            p.dma("sp", xo[sl][:, :], I["x"][i * 128:(i + 1) * 128, :], Bxo[sl], writes=[Bxo[sl]])
            tt(p, "dve", xo[sl][:, :], xo[sl][:, :], sc[:, :], ALU.mult, [Bxo[sl], Bsc], [Bxo[sl]])
            tt(p, "pool", hb[sl][:, :], xo[sl][:, :], sh[:, :], ALU.add, [Bxo[sl], Bsh], [Bhb[sl]])
            emit_ht(k, H, hb[sl], Bhb[sl], i)
        p.flush()


def phase_epilogue(k, s, last, first):
    p, nc, I = k.p, k.nc, k.I
    l, j = divmod(s, 2)
    nxt_moe = (not last) and ((s + 1) % 2 == 1) and (((s + 1) // 2) % 2 == 1)
    with ExitStack() as st:
        names = ["gate", "lng", "lnb", "Gp", "Bp"]
        V = {n: sbt(k, st, "ev_" + n, [128, D], F32) for n in names}
        BV = {n: Buf("ev_" + n) for n in names}
        load_bc(k, p, V["gate"], BV["gate"], ada_slice(k, l, 3 * j + 2))
        load_bc(k, p, V["lng"], BV["lng"], I["ln_g"][l, j, :])
        load_bc(k, p, V["lnb"], BV["lnb"], I["ln_b"][l, j, :])
        if not last:
            l2, j2 = divmod(s + 1, 2)
            load_bc(k, p, V["Gp"], BV["Gp"], ada_slice(k, l2, 3 * j2 + 1))
            load_bc(k, p, V["Bp"], BV["Bp"], ada_slice(k, l2, 3 * j2 + 0))
            tsc(p, "dve", V["Gp"][:, :], V["Gp"][:, :], 1.0, ALU.add, [BV["Gp"]], [BV["Gp"]])
            tmpv = sbt(k, st, "ev_tmp", [128, D], F32)
            Btmp = Buf("ev_tmp")
            tt(p, "dve", tmpv[:, :], V["lnb"][:, :], V["Gp"][:, :], ALU.mult, [BV["lnb"], BV["Gp"]], [Btmp])
            tt(p, "dve", V["Bp"][:, :], V["Bp"][:, :], tmpv[:, :], ALU.add, [BV["Bp"], Btmp], [BV["Bp"]])
            tt(p, "dve", V["Gp"][:, :], V["Gp"][:, :], V["lng"][:, :], ALU.mult, [BV["Gp"], BV["lng"]], [BV["Gp"]])
        xo = [sbt(k, st, "e_xo%d" % i, [128, D], F32) for i in range(2)]
        sb = [sbt(k, st, "e_sb%d" % i, [128, D], F32) for i in range(2)]
        xh = sbt(k, st, "e_xh", [128, D], F32)
        xn = [sbt(k, st, "e_xn%d" % i, [128, D], F32) for i in range(2)]
        hb = [sbt(k, st, "e_hb%d" % i, [128, D], BF16) for i in range(2)]
        st6 = sbt(k, st, "e_st6", [128, 2, 6], F32)
        mv = sbt(k, st, "e_mv", [128, 2], F32)
        rstd = sbt(k, st, "e_rstd", [128, 1], F32)
        Bxo, Bsb, Bxn, Bhb = [Buf("a"), Buf("b")], [Buf("a"), Buf("b")], [Buf("a"), Buf("b")], [Buf("a"), Buf("b")]
        Bxh, Bst6, Bmv, Brs = Buf("xh"), Buf("st6"), Buf("mv"), Buf("rstd")
        H = HT(k, st, "e")
        if nxt_moe:
            li = (s + 1) // 4
            wr = sbt(k, st, "e_wr", [128, 8, 8], F32)
            wrh = sbt(k, st, "e_wrh", [128, 8, 8], BF16)
            wrl = sbt(k, st, "e_wrl", [128, 8, 8], BF16)
            hlo = [sbt(k, st, "e_hlo%d" % i, [128, D], BF16) for i in range(2)]
            hloT = sbt(k, st, "e_hloT", [128, 8, 128], BF16)
            lg = sbt(k, st, "e_lg", [128, 8], F32)
            lg2 = sbt(k, st, "e_lg2", [128, 8], F32)
            m1 = sbt(k, st, "e_m1", [128, 1], F32)
            m2 = sbt(k, st, "e_m2", [128, 1], F32)
            msk1 = sbt(k, st, "e_msk1", [128, 8], F32)
            msk2 = sbt(k, st, "e_msk2", [128, 8], F32)
            g1 = sbt(k, st, "e_g1", [128, 1], F32)
            g2 = sbt(k, st, "e_g2", [128, 1], F32)
            comb = [sbt(k, st, "e_comb%d" % i, [128, 8], F32) for i in range(2)]
            Bwr, Bwrh, Bwrl = Buf("wr"), Buf("wrh"), Buf("wrl")
            Bhlo, BhloT = [Buf("a"), Buf("b")], Buf("hloT")
            Brt = Buf("router_tmp")
            Bcomb = [Buf("a"), Buf("b")]
            p.dma("sp", wr[:, :, :], I["w_router"][li].rearrange("(k p) e -> p k e", p=128), Bwr, writes=[Bwr])
            cpy(p, "dve", wrh[:, :, :], wr[:, :, :], [Bwr], [Bwrh])
            tt(p, "dve", wr[:, :, :], wr[:, :, :], wrh[:, :, :], ALU.subtract, [Bwr, Bwrh], [Bwr])
            cpy(p, "dve", wrl[:, :, :], wr[:, :, :], [Bwr], [Bwrl])
        xsrc = I["x"] if first else k.xres
        xdst = k.out if last else k.xres
        for i in range(NT):
            sl = i % 2
            rows = slice(i * 128, (i + 1) * 128)
            p.dma("sp", xo[sl][:, :], xsrc[rows, :], Bxo[sl], reads=[k.B_x[i]], writes=[Bxo[sl]])
            p.dma("sp", sb[sl][:, :], k.sub_d[rows, :], Bsb[sl], reads=[k.B_sub[i]], writes=[Bsb[sl]])
            tt(p, "dve", sb[sl][:, :], sb[sl][:, :], V["gate"][:, :], ALU.mult, [Bsb[sl], BV["gate"]], [Bsb[sl]])
            stt(p, "pool", xo[sl][:, :], xo[sl][:, :], ALPHA, sb[sl][:, :], ALU.mult, ALU.add,
                [Bxo[sl], Bsb[sl]], [Bxo[sl]])
            for hh in range(2):
                p.op("dve", (lambda e, hh=hh, sl=sl: e.bn_stats(out=st6[:, hh, :], in_=xo[sl][:, hh * 512:(hh + 1) * 512])),
                     [Bxo[sl]], [Bst6])
            p.op("dve", lambda e: e.bn_aggr(out=mv[:, :], in_=st6[:, :, :]), [Bst6], [Bmv], sync_same=True)
            tsc(p, "dve", rstd[:, :], mv[:, 1:2], LN_EPS, ALU.add, [Bmv], [Brs], ss=True)
            p.op("act", lambda e: e.sqrt(out=rstd[:, :], in_=rstd[:, :]), [Brs], [Brs])
            p.op("dve", lambda e: e.reciprocal(out=rstd[:, :], in_=rstd[:, :]), [Brs], [Brs])
            tsc(p, "dve", xh[:, :], xo[sl][:, :], mv[:, 0:1], ALU.subtract, [Bxo[sl], Bmv, Brs], [Bxh],
                s2=rstd[:, 0:1], op1=ALU.mult, ss=True)
            tt(p, "pool", xn[sl][:, :], xh[:, :], V["lng"][:, :], ALU.mult, [Bxh, BV["lng"]], [Bxn[sl]])
            tt(p, "pool", xn[sl][:, :], xn[sl][:, :], V["lnb"][:, :], ALU.add, [Bxn[sl], BV["lnb"]], [Bxn[sl]])
            p.dma("sp", xdst[rows, :], xn[sl][:, :], Bxn[sl], reads=[Bxn[sl]], writes=[k.B_x[i]])
            if last:
                continue
            tt(p, "dve", xh[:, :], xh[:, :], V["Gp"][:, :], ALU.mult, [Bxh, BV["Gp"]], [Bxh])
            if not nxt_moe:
                tt(p, "dve", hb[sl][:, :], xh[:, :], V["Bp"][:, :], ALU.add, [Bxh, BV["Bp"]], [Bhb[sl]])
                emit_ht(k, H, hb[sl], Bhb[sl], i)
            else:
                tt(p, "dve", xh[:, :], xh[:, :], V["Bp"][:, :], ALU.add, [Bxh, BV["Bp"]], [Bxh])
                cpy(p, "act", hb[sl][:, :], xh[:, :], [Bxh], [Bhb[sl]])
                tt(p, "pool", hlo[sl][:, :], xh[:, :], hb[sl][:, :], ALU.subtract, [Bxh, Bhb[sl]], [Bhlo[sl]])
                emit_ht(k, H, hb[sl], Bhb[sl], i)
                c4, j4 = divmod(i, 4)
                hsl = c4 % 2
                for kk in range(8):
                    trp(p, H.pst[:, kk * 128:(kk + 1) * 128], hlo[sl][:, kk * 128:(kk + 1) * 128], k.ident[:, :],
                        [Bhlo[sl], k.Bident], [H.Bp])
                cpy(p, "act", hloT[:, :, :], H.pst[:, :].rearrange("p (k t) -> p k t", k=8), [H.Bp], [BhloT])
                bank = 6
                terms = []
                for kk in range(8):
                    hi = H.hst[hsl][:, kk, j4 * 128:(j4 + 1) * 128]
                    terms.append((hi, wrh[:, kk, :], [H.B[hsl], Bwrh]))
                    terms.append((hloT[:, kk, :], wrh[:, kk, :], [BhloT, Bwrh]))
                    terms.append((hi, wrl[:, kk, :], [H.B[hsl], Bwrl]))
                for ti, (lh, rh, rd) in enumerate(terms):
                    mm(p, k.ps[bank][:, 0:8], lh, rh, ti == 0, ti == len(terms) - 1, rd, [k.Bps[bank]])
                cpy(p, "dve", lg[:, :], k.ps[bank][:, 0:8], [k.Bps[bank]], [Brt])
                R = [Brt]
                p.op("dve", lambda e: e.reduce_max(out=m1[:, :], in_=lg[:, :], axis=mybir.AxisListType.X), R, R, sync_same=True)
                tsc(p, "dve", msk1[:, :], lg[:, :], m1[:, 0:1], ALU.is_equal, R, R, ss=True)
                stt(p, "dve", lg2[:, :], msk1[:, :], -1e30, lg[:, :], ALU.mult, ALU.add, R, R, ss=True)
                p.op("dve", lambda e: e.reduce_max(out=m2[:, :], in_=lg2[:, :], axis=mybir.AxisListType.X), R, R, sync_same=True)
                tsc(p, "dve", msk2[:, :], lg2[:, :], m2[:, 0:1], ALU.is_equal, R, R, ss=True)
                tt(p, "dve", g2[:, :], m2[:, :], m1[:, :], ALU.subtract, R, R, ss=True)
                actf(p, g2[:, :], g2[:, :], AF.Sigmoid, R, R)
                tsc(p, "dve", g1[:, :], g2[:, :], -1.0, ALU.mult, R, R, s2=1.0, op1=ALU.add, ss=True)
                tsc(p, "dve", msk1[:, :], msk1[:, :], g1[:, 0:1], ALU.mult, R, R, ss=True)
                stt(p, "dve", comb[sl][:, :], msk2[:, :], g2[:, 0:1], msk1[:, :], ALU.mult, ALU.add,
                    R + [Bcomb[sl]], R + [Bcomb[sl]], ss=True)
                p.dma("sp", k.comb_d[rows, :], comb[sl][:, :], Bcomb[sl], reads=[Bcomb[sl]], writes=[k.B_comb])
        p.flush()


def ffn_phase(k, li, moe):
    p, nc, I = k.p, k.nc, k.I
    TC = 1024
    with ExitStack() as st:
        hTc = [sbt(k, st, "f_hT%d" % i, [128, 8, TC], BF16) for i in range(2)]
        BhT = [Buf("a"), Buf("b")]
        actT = sbt(k, st, "f_act", [128, NFF, TC], BF16)
        Bact = [Buf("act%d" % j) for j in range(NFF)]
        wd = [sbt(k, st, "f_wd%d" % i, [128, NFF, 512], BF16) for i in range(2)]
        Bwd = [Buf("a"), Buf("b")]
        acc = sbt(k, st, "f_acc", [128, 8, D], F32)
        Bacc = [Buf("acc%d" % t) for t in range(8)]
        NS = 3
        slg = [sbt(k, st, "f_slg%d" % i, [128, 8, 256], BF16) for i in range(NS)]
        slu = [sbt(k, st, "f_slu%d" % i, [128, 8, 256], BF16) for i in range(NS)]
        Bsl = [Buf("sl%d" % i) for i in range(NS)]
        sg = [sbt(k, st, "f_sg%d" % i, [128, 512], F32) for i in range(2)]
        Bsg = [Buf("a"), Buf("b")]
        comb = sbt(k, st, "f_comb", [128, 8, 8], F32)
        Bcomb = Buf("comb")
        hT_v = k.hT_d.rearrange("(k p) t -> p k t", p=128)
        cnt_sl = 0
        cnt_gu = 0
        cnt_dn = 0
        ne = 8 if moe else 1
        for tcx in range(S // TC):
            hs = tcx % 2
            p.dma("sp", hTc[hs][:, :, :], hT_v[:, :, tcx * TC:(tcx + 1) * TC], BhT[hs],
                  reads=[k.B_hT[2 * tcx], k.B_hT[2 * tcx + 1]], writes=[BhT[hs]])
            if moe:
                p.dma("sp", comb[:, :, :],
                      k.comb_d[tcx * TC:(tcx + 1) * TC, :].rearrange("(t p) e -> p t e", p=128),
                      Bcomb, reads=[k.B_comb], writes=[Bcomb])
            for e in range(ne):
                wgu = I["w_exp_gu"][li, e] if moe else I["w_ffn_gu"][li]
                wdn = I["w_exp_down"][li, e] if moe else I["w_ffn_down"][li]
                for si in range(11):
                    slot = cnt_sl % NS
                    cnt_sl += 1
                    p.dma("pool", slg[slot][:, :, :],
                          wgu[:, si * 256:(si + 1) * 256].rearrange("(k p) n -> p k n", p=128),
                          Bsl[slot], writes=[Bsl[slot]])
                    p.dma("pool", slu[slot][:, :, :],
                          wgu[:, DFF + si * 256:DFF + (si + 1) * 256].rearrange("(k p) n -> p k n", p=128),
                          Bsl[slot], writes=[Bsl[slot]])
                    if si in (2, 6):
                        nh = 0 if si == 2 else 1
                        p.dma("pool", wd[nh][:, :, :],
                              wdn[:, nh * 512:(nh + 1) * 512].rearrange("(j p) n -> p j n", p=128),
                              Bwd[nh], writes=[Bwd[nh]])
                    for jj in range(2):
                        j = si * 2 + jj
                        for tc in range(TC // 512):
                            bg = (cnt_gu % 2) * 2
                            bu = bg + 1
                            ss = cnt_gu % 2
                            cnt_gu += 1
                            tok = slice(tc * 512, (tc + 1) * 512)
                            for kk in range(8):
                                mm(p, k.ps[bg][:, :], slg[slot][:, kk, jj * 128:(jj + 1) * 128], hTc[hs][:, kk, tok],
                                   kk == 0, kk == 7, [Bsl[slot], BhT[hs]], [k.Bps[bg]])
                            for kk in range(8):
                                mm(p, k.ps[bu][:, :], slu[slot][:, kk, jj * 128:(jj + 1) * 128], hTc[hs][:, kk, tok],
                                   kk == 0, kk == 7, [Bsl[slot], BhT[hs]], [k.Bps[bu]])
                            actf(p, sg[ss][:, :], k.ps[bg][:, :], AF.Silu, [k.Bps[bg]], [Bsg[ss]])
                            tt(p, "dve", actT[:, j, tok], sg[ss][:, :], k.ps[bu][:, :], ALU.mult,
                               [Bsg[ss], k.Bps[bu]], [Bact[j]])
                for nh in range(2):
                    for t in range(8):
                        bank = 4 + cnt_dn % 2
                        cnt_dn += 1
                        for j in range(NFF):
                            mm(p, k.ps[bank][:, :], actT[:, j, t * 128:(t + 1) * 128], wd[nh][:, j, :],
                               j == 0, j == NFF - 1, [Bact[j], Bwd[nh]], [k.Bps[bank]])
                        dst = acc[:, t, nh * 512:(nh + 1) * 512]
                        if not moe:
                            cpy(p, "dve", dst, k.ps[bank][:, :], [k.Bps[bank]], [Bacc[t]])
                        elif e == 0:
                            tsc(p, "dve", dst, k.ps[bank][:, :], comb[:, t, e:e + 1], ALU.mult,
                                [k.Bps[bank], Bcomb], [Bacc[t]])
                        else:
                            stt(p, "dve", dst, k.ps[bank][:, :], comb[:, t, e:e + 1], dst, ALU.mult, ALU.add,
                                [k.Bps[bank], Bcomb, Bacc[t]], [Bacc[t]])
            for t in range(8):
                i = tcx * 8 + t
                p.dma("sp", k.sub_d[i * 128:(i + 1) * 128, :], acc[:, t, :], Bacc[t], reads=[Bacc[t]],
                      writes=[k.B_sub[i]])
        p.flush()


def ffn_layer(k, l, s, last, moe):
    ffn_phase(k, l // 2, moe)
    phase_epilogue(k, s, last, first=(s == k.s0))


def outproj_phase(k, w_ap, nk):
    p, nc, I = k.p, k.nc, k.I
    with ExitStack() as st:
        w = sbt(k, st, "o_w", [128, nk, D], BF16)
        Bw = Buf("o_w")
        mx = [sbt(k, st, "o_mx%d" % i, [128, nk, 512], BF16) for i in range(2)]
        Bmx = [Buf("a"), Buf("b")]
        so = [sbt(k, st, "o_so%d" % i, [128, D], F32) for i in range(2)]
        Bso = [Buf("a"), Buf("b")]
        p.dma("pool", w[:, :, :], w_ap.rearrange("(k p) n -> p k n", p=128), Bw, writes=[Bw])
        mix_v = k.mixT_d.rearrange("(k p) t -> p k t", p=128)
        for c in range(8):
            ms = c % 2
            p.dma("sp", mx[ms][:, :, :], mix_v[:, 0:nk, c * 512:(c + 1) * 512], Bmx[ms],
                  reads=[k.B_mix[c]], writes=[Bmx[ms]])
            for t in range(4):
                i = c * 4 + t
                sl = i % 2
                for nh in range(2):
                    bank = (i * 2 + nh) % 4
                    for kk in range(nk):
                        mm(p, k.ps[bank][:, :], mx[ms][:, kk, t * 128:(t + 1) * 128], w[:, kk, nh * 512:(nh + 1) * 512],
                           kk == 0, kk == nk - 1, [Bmx[ms], Bw], [k.Bps[bank]])
                    if nh == 0:
                        cpy(p, "dve", so[sl][:, 0:512], k.ps[bank][:, :], [k.Bps[bank]], [Bso[sl]])
                    else:
                        cpy(p, "act", so[sl][:, 512:1024], k.ps[bank][:, :], [k.Bps[bank]], [Bso[sl]])
                p.dma("sp", k.sub_d[i * 128:(i + 1) * 128, :], so[sl][:, :], Bso[sl], reads=[Bso[sl]],
                      writes=[k.B_sub[i]])
        p.flush()


def fox_proj_phase(k, li):
    p, nc, I = k.p, k.nc, k.I
    with ExitStack() as st:
        w = sbt(k, st, "x_w", [128, 8, 3088], BF16)
        Bw = [Buf("w%d" % i) for i in range(4)]
        hTc = [sbt(k, st, "x_hT%d" % i, [128, 8, 512], BF16) for i in range(2)]
        BhT = [Buf("a"), Buf("b")]
        qst = [sbt(k, st, "x_qst%d" % i, [128, 512], BF16) for i in range(3)]
        Bqst = [Buf("a"), Buf("b"), Buf("c")]
        vst = [sbt(k, st, "x_vst%d" % i, [128, 16, 65], BF16) for i in range(2)]
        Bvst = [Buf("a"), Buf("b")]
        bf = sbt(k, st, "x_bf", [16, 1], F32)
        Bbf = Buf("bf")
        G = sbt(k, st, "x_G", [16, S], F32)
        BG = Buf("G")
        ex = sbt(k, st, "x_ex", [16, 512], F32)
        Bex = Buf("ex")
        onef = sbt(k, st, "x_onef", [16, 512], F32)
        oneb = sbt(k, st, "x_oneb", [16, 512], BF16)
        Bone = Buf("one")
        rr = sbt(k, st, "x_rr", [16, 512], F32)
        Brr = Buf("rr")
        gp = [[sbt(k, st, "x_gp%d_%d" % (i, r), [16, 512], BF16) for r in range(3)] for i in range(2)]
        gn = [[sbt(k, st, "x_gn%d_%d" % (i, r), [16, 512], BF16) for r in range(3)] for i in range(2)]
        Bgp = [Buf("a"), Buf("b")]
        win = I["w_in_odd"][li]
        pieces = [(0, 1024), (1024, 2048), (2048, 3072), (3072, 3088)]
        for pi, (a, b) in enumerate(pieces):
            p.dma("pool", w[:, :, a:b], win[:, a:b].rearrange("(k p) n -> p k n", p=128), Bw[pi], writes=[Bw[pi]])
        with_b = I["b_forget"][li].rearrange("(h o) -> h o", o=1)
        p.dma("sp", bf[:, :], with_b, Bbf, writes=[Bbf])
        tsc(p, "dve", bf[:, :], bf[:, :], -1.0, ALU.mult, [Bbf], [Bbf])
        mset(p, "dve", onef[:, :], 1.0, [Bone])
        mset(p, "dve", oneb[:, :], 1.0, [Bone])
        for i in range(2):
            mset(p, "pool", vst[i][:, :, 64:65], 1.0, [Bvst[i]])
        hT_v = k.hT_d.rearrange("(k p) t -> p k t", p=128)
        cq = 0
        cv = 0
        for c in range(8):
            hs = c % 2
            tok = slice(c * 512, (c + 1) * 512)
            p.dma("sp", hTc[hs][:, :, :], hT_v[:, :, tok], BhT[hs], reads=[k.B_hT[c]], writes=[BhT[hs]])
            for sec in range(2):
                dst = k.qa_d if sec == 0 else k.ka_d
                for jb in range(8):
                    bank = cq % 3
                    qs = cq % 3
                    cq += 1
                    col = sec * 1024 + jb * 128
                    for kk in range(8):
                        mm(p, k.ps[bank][:, :], w[:, kk, col:col + 128], hTc[hs][:, kk, :], kk == 0, kk == 7,
                           [Bw[sec], BhT[hs]], [k.Bps[bank]])
                    if sec == 0:
                        p.op("act", (lambda e, o=qst[qs][:, :], i_=k.ps[bank][:, :]: e.mul(out=o, in_=i_, mul=0.125)),
                             [k.Bps[bank]], [Bqst[qs]])
                    else:
                        cpy(p, "dve", qst[qs][:, :], k.ps[bank][:, :], [k.Bps[bank]], [Bqst[qs]])
                    for hh in range(2):
                        p.dma("sp", dst[2 * jb + hh, 0:64, tok], qst[qs][hh * 64:(hh + 1) * 64, :], Bqst[qs],
                              reads=[Bqst[qs]], writes=[k.B_qk])
            for t in range(4):
                vs = cv % 2
                cv += 1
                for nh in range(2):
                    bank = 3 + nh
                    for kk in range(8):
                        mm(p, k.ps[bank][:, :], hTc[hs][:, kk, t * 128:(t + 1) * 128],
                           w[:, kk, 2048 + nh * 512:2048 + (nh + 1) * 512], kk == 0, kk == 7,
                           [BhT[hs], Bw[2]], [k.Bps[bank]])
                    eng = "act" if nh == 0 else "dve"
                    cpy(p, eng, vst[vs][:, nh * 8:(nh + 1) * 8, 0:64],
                        k.ps[bank][:, :].rearrange("p (h d) -> p h d", d=64), [k.Bps[bank]], [Bvst[vs]])
                i = c * 4 + t
                p.dma("sp", k.va_d[i * 128:(i + 1) * 128, :], vst[vs][:, :, :].rearrange("p h d -> p (h d)"),
                      Bvst[vs], reads=[Bvst[vs]], writes=[k.B_va])
            bank = 5
            for kk in range(8):
                mm(p, k.ps[bank][0:16, :], w[:, kk, 3072:3088], hTc[hs][:, kk, :], kk == 0, kk == 7,
                   [Bw[3], BhT[hs]], [k.Bps[bank]])
            actf(p, ex[:, :], k.ps[bank][0:16, :], AF.Exp, [k.Bps[bank], Bbf], [Bex], bias=bf[:, 0:1], scale=-1.0)
            actf(p, ex[:, :], ex[:, :], AF.Ln, [Bex], [Bex], bias=1.0)
            init = 0.0 if c == 0 else G[:, c * 512 - 1:c * 512]
            p.op("dve", (lambda e, init=init, tok=tok: e.tensor_tensor_scan(
                out=G[:, tok], data0=onef[:, :], data1=ex[:, :], initial=init, op0=ALU.mult, op1=ALU.add)),
                [Bex, Bone, BG], [BG])
            gs = c % 2
            cpy(p, "dve", gp[gs][0][:, :], G[:, tok], [BG], [Bgp[gs]])
            tt(p, "dve", rr[:, :], G[:, tok], gp[gs][0][:, :], ALU.subtract, [BG, Bgp[gs]], [Brr])
            cpy(p, "dve", gp[gs][1][:, :], rr[:, :], [Brr], [Bgp[gs]])
            tt(p, "dve", rr[:, :], rr[:, :], gp[gs][1][:, :], ALU.subtract, [Brr, Bgp[gs]], [Brr])
            cpy(p, "dve", gp[gs][2][:, :], rr[:, :], [Brr], [Bgp[gs]])
            for r in range(3):
                tsc(p, "dve", gn[gs][r][:, :], gp[gs][r][:, :], -1.0, ALU.mult, [Bgp[gs]], [Bgp[gs]])
            for r in range(3):
                p.dma("sp", k.qa_d[:, 64 + r, tok], gn[gs][r][:, :], Bgp[gs], reads=[Bgp[gs]], writes=[k.B_qk])
                p.dma("sp", k.qa_d[:, 67 + r, tok], oneb[:, :], Bgp[gs], reads=[Bone], writes=[k.B_qk])
                p.dma("sp", k.ka_d[:, 64 + r, tok], oneb[:, :], Bgp[gs], reads=[Bone], writes=[k.B_qk])
                p.dma("sp", k.ka_d[:, 67 + r, tok], gp[gs][r][:, :], Bgp[gs], reads=[Bgp[gs]], writes=[k.B_qk])
        p.flush()


def fox_attn_phase(k):
    p, nc, I = k.p, k.nc, k.I
    with ExitStack() as st:
        qa = [sbt(k, st, "a_qa%d" % i, [70, S], BF16) for i in range(2)]
        ka = [sbt(k, st, "a_ka%d" % i, [70, S], BF16) for i in range(2)]
        Bqk = [Buf("a"), Buf("b")]
        va = sbt(k, st, "a_va", [128, NT, 16 * 65], BF16)
        Bva = Buf("va")
        NP = 3
        pt = [sbt(k, st, "a_pt%d" % i, [128, 512], BF16) for i in range(NP)]
        Bpt = [Buf("pt%d" % i) for i in range(NP)]
        maskc = sbt(k, st, "a_maskc", [128, 128], BF16)
        Bmask = Buf("maskc")
        rden = sbt(k, st, "a_rden", [65, 512], F32)
        Brden = Buf("rden")
        onesf = sbt(k, st, "a_onesf", [65, 64], F32)
        Bones = Buf("onesf")
        bcs = [sbt(k, st, "a_bcs%d" % i, [64, 512], F32) for i in range(2)]
        Bbcs = [Buf("a"), Buf("b")]
        ost = [sbt(k, st, "a_ost%d" % i, [64, 512], BF16) for i in range(2)]
        Bost = [Buf("a"), Buf("b")]
        p.dma("sp", maskc[:, :], I["maskcur"][:, :], Bmask, writes=[Bmask])
        mset(p, "dve", onesf[:, :], 1.0, [Bones])
        for g4 in range(4):
            p.dma("sp", va[:, g4 * 8:(g4 + 1) * 8, :],
                  k.va_d[g4 * 1024:(g4 + 1) * 1024, :].rearrange("(t p) f -> p t f", p=128), Bva,
                  reads=[k.B_va], writes=[Bva])
        cst = 0
        cch = 0
        for h in range(16):
            hs = h % 2
            p.dma("sp", qa[hs][:, :], k.qa_d[h, :, :], Bqk[hs], reads=[k.B_qk], writes=[Bqk[hs]])
            p.dma("sp", ka[hs][:, :], k.ka_d[h, :, :], Bqk[hs], reads=[k.B_qk], writes=[Bqk[hs]])
            for c in range(8):
                ob = 4 + cch % 2
                sl = cch % 2
                cch += 1
                nkt = 4 * c + 4
                slots = {}

                def emit_st(kt):
                    nonlocal cst
                    s_ = cst % NP
                    cst += 1
                    slots[kt] = s_
                    col0 = max(0, kt - 4 * c) * 128
                    lh = ka[hs][0:70, kt * 128:(kt + 1) * 128]
                    if kt < 4 * c:
                        mm(p, k.ps[s_][:, :], lh, qa[hs][0:70, c * 512:(c + 1) * 512], True, True,
                           [Bqk[hs]], [k.Bps[s_]])
                    else:
                        mm(p, k.ps[s_][:, col0:col0 + 128], lh, qa[hs][0:70, c * 512 + col0:c * 512 + col0 + 128],
                           True, False, [Bqk[hs]], [k.Bps[s_]])
                        mm(p, k.ps[s_][:, col0:col0 + 128], k.ident[:, :], maskc[:, :], False, True,
                           [k.Bident, Bmask], [k.Bps[s_]])
                        if col0 + 128 < 512:
                            mm(p, k.ps[s_][:, col0 + 128:512], lh, qa[hs][0:70, c * 512 + col0 + 128:(c + 1) * 512],
                               True, True, [Bqk[hs]], [k.Bps[s_]])

                emit_st(0)
                if nkt > 1:
                    emit_st(1)
                for kt in range(nkt):
                    s_ = slots[kt]
                    col0 = max(0, kt - 4 * c) * 128
                    actf(p, pt[s_][:, col0:512], k.ps[s_][:, col0:512], AF.Exp, [k.Bps[s_]], [Bpt[s_]])
                    if kt + 2 < nkt:
                        emit_st(kt + 2)
                    mm(p, k.ps[ob][0:65, col0:512], va[:, kt, h * 65:(h + 1) * 65], pt[s_][:, col0:512],
                       kt == 0, kt == nkt - 1, [Bva, Bpt[s_]], [k.Bps[ob]])
                p.op("dve", (lambda e, ob=ob: e.reciprocal(out=rden[64:65, :], in_=k.ps[ob][64:65, :])),
                     [k.Bps[ob]], [Brden])
                mm(p, k.ps[6][0:64, :], onesf[64:65, 0:64], rden[64:65, :], True, True, [Bones, Brden], [k.Bps[6]])
                cpy(p, "act", bcs[sl][:, :], k.ps[6][0:64, :], [k.Bps[6]], [Bbcs[sl]])
                tt(p, "dve", ost[sl][:, :], k.ps[ob][0:64, :], bcs[sl][:, :], ALU.mult, [k.Bps[ob], Bbcs[sl]],
                   [Bost[sl]])
                p.dma("sp", k.mixT_d[h * 64:(h + 1) * 64, c * 512:(c + 1) * 512], ost[sl][:, :], Bost[sl],
                      reads=[Bost[sl]], writes=[k.B_mix[c]])
        p.flush()


def odd_mixer(k, l, s, last):
    li = l // 2
    fox_proj_phase(k, li)
    fox_attn_phase(k)
    outproj_phase(k, k.I["w_out_odd"][li], 8)
    phase_epilogue(k, s, last, first=(s == k.s0))


def even_pool_phase(k, li):
    p, nc, I = k.p, k.nc, k.I
    win = I["w_in_even"][li]
    with ExitStack() as st:
        w = sbt(k, st, "p_w", [128, 8, 256], BF16)
        Bw = Buf("w")
        hTc = [sbt(k, st, "p_hT%d" % i, [128, 8, 512], BF16) for i in range(2)]
        BhT = [Buf("a"), Buf("b")]
        U = sbt(k, st, "p_U", [128, 2, S], F32)
        S1 = sbt(k, st, "p_S1", [128, 2, S], F32)
        S2 = sbt(k, st, "p_S2", [128, 2, S], F32)
        S3 = sbt(k, st, "p_S3", [128, S], F32)
        S4 = sbt(k, st, "p_S4", [128, S], F32)
        PL = sbt(k, st, "p_PL", [128, 2, S], BF16)
        BU, BS1, BS2, BS3, BS4, BPL = Buf("U"), Buf("S1"), Buf("S2"), Buf("S3"), Buf("S4"), Buf("PL")
        wp = sbt(k, st, "p_wp", [128, 2, 128], BF16)
        Bwp = Buf("wp")
        psc = sbt(k, st, "p_psc", [128, 2], F32)
        invw = sbt(k, st, "p_invw", [128, 2], F32)
        invc = sbt(k, st, "p_invc", [128, 2, 16], F32)
        t16 = sbt(k, st, "p_t16", [128, 16], F32)
        Bc = Buf("consts")
        Bt16 = Buf("t16")
        ost = [sbt(k, st, "p_ost%d" % i, [128, 512], BF16) for i in range(2)]
        Bost = [Buf("a"), Buf("b")]
        p.dma("pool", w[:, :, :], win[:, 0:256].rearrange("(k p) n -> p k n", p=128), Bw, writes=[Bw])
        mset(p, "dve", wp[:, :, :], 0.0, [Bwp])
        for g in range(4):
            blk, pr = divmod(g, 2)
            p.dma("pool", wp[pr * 64:(pr + 1) * 64, blk, pr * 64:(pr + 1) * 64], I["w_pool"][li, g], Bwp, writes=[Bwp])
        for blk in range(2):
            p.dma("sp", psc[:, blk:blk + 1],
                  I["pool_scale"][li, blk * 128:(blk + 1) * 128].rearrange("(p o) -> p o", o=1), Bc, writes=[Bc])
        p.dma("sp", invw[:, :], I["invw"][:, :], Bc, writes=[Bc])
        p.dma("sp", invc[:, :, :], I["invc"][:, :, :], Bc, writes=[Bc])
        hT_v = k.hT_d.rearrange("(k p) t -> p k t", p=128)
        for c in range(8):
            hs = c % 2
            tok = slice(c * 512, (c + 1) * 512)
            p.dma("sp", hTc[hs][:, :, :], hT_v[:, :, tok], BhT[hs], reads=[k.B_hT[c]], writes=[BhT[hs]])
            for blk in range(2):
                bank = (c * 2 + blk) % 4
                for kk in range(8):
                    mm(p, k.ps[bank][:, :], w[:, kk, blk * 128:(blk + 1) * 128], hTc[hs][:, kk, :], kk == 0, kk == 7,
                       [Bw, BhT[hs]], [k.Bps[bank]])
                cpy(p, "act" if blk == 0 else "dve", U[:, blk, tok], k.ps[bank][:, :], [k.Bps[bank]], [BU])
        tt(p, "dve", S1[:, :, 1:S], U[:, :, 1:S], U[:, :, 0:S - 1], ALU.add, [BU], [BS1], ss=True)
        cpy(p, "dve", S1[:, :, 0:1], U[:, :, 0:1], [BU], [BS1], ss=True)
        tt(p, "pool", S2[64:128, 0, 2:S], S1[64:128, 0, 2:S], S1[64:128, 0, 0:S - 2], ALU.add, [BS1], [BS2], ss=True)
        cpy(p, "pool", S2[64:128, 0, 0:2], S1[64:128, 0, 0:2], [BS1], [BS2], ss=True)
        tt(p, "dve", S2[:, 1, 2:S], S1[:, 1, 2:S], S1[:, 1, 0:S - 2], ALU.add, [BS1], [BS2], ss=True)
        cpy(p, "dve", S2[:, 1, 0:2], S1[:, 1, 0:2], [BS1], [BS2], ss=True)
        tt(p, "dve", S3[:, 4:S], S2[:, 1, 4:S], S2[:, 1, 0:S - 4], ALU.add, [BS2], [BS3], ss=True)
        cpy(p, "dve", S3[:, 0:4], S2[:, 1, 0:4], [BS2], [BS3], ss=True)
        tt(p, "dve", S4[64:128, 8:S], S3[64:128, 8:S], S3[64:128, 0:S - 8], ALU.add, [BS3], [BS4], ss=True)
        cpy(p, "dve", S4[64:128, 0:8], S3[64:128, 0:8], [BS3], [BS4], ss=True)
        srcs = [(S1, BS1, lambda t, a, b: t[0:64, 0, a:b]), (S2, BS2, lambda t, a, b: t[64:128, 0, a:b]),
                (S3, BS3, lambda t, a, b: t[0:64, a:b]), (S4, BS4, lambda t, a, b: t[64:128, a:b])]
        for g, (T, BT, view) in enumerate(srcs):
            blk, pr = divmod(g, 2)
            ps_ = slice(pr * 64, (pr + 1) * 64)
            eng = "dve" if g % 2 == 0 else "pool"
            stt(p, eng, PL[ps_, blk, 16:S], view(T, 16, S), invw[ps_, blk:blk + 1], U[ps_, blk, 16:S],
                ALU.mult, ALU.subtract, [BT, BU, Bc], [BPL], ss=True)
            tt(p, "dve", t16[ps_, :], view(T, 0, 16), invc[ps_, blk, :], ALU.mult, [BT, Bc], [Bt16], ss=True)
            tt(p, "dve", PL[ps_, blk, 0:16], t16[ps_, :], U[ps_, blk, 0:16], ALU.subtract, [Bt16, BU], [BPL], ss=True)
        for c in range(8):
            tok = slice(c * 512, (c + 1) * 512)
            for blk in range(2):
                n = c * 2 + blk
                bank = n % 4
                sl = n % 2
                mm(p, k.ps[bank][:, :], wp[:, blk, :], PL[:, blk, tok], True, True, [Bwp, BPL], [k.Bps[bank]])
                tsc(p, "dve", ost[sl][:, :], k.ps[bank][:, :], psc[:, blk:blk + 1], ALU.mult, [k.Bps[bank], Bc],
                    [Bost[sl]])
                p.dma("sp", k.mixT_d[blk * 128:(blk + 1) * 128, tok], ost[sl][:, :], Bost[sl], reads=[Bost[sl]],
                      writes=[k.B_mix[c]])
        p.flush()


DIL = [1, 4, 16]


def even_attn_phase(k, li):
    p, nc, I = k.p, k.nc, k.I
    win = I["w_in_even"][li]
    with ExitStack() as st:
        acc = sbt(k, st, "d_acc", [65, 4, S], F32)
        Bacc = [Buf("acc%d" % h) for h in range(4)]
        rC = sbt(k, st, "d_rC", [128, S], F32)
        rS = sbt(k, st, "d_rS", [128, S], F32)
        RT = sbt(k, st, "d_RT", [128, 128], BF16)
        mcur = sbt(k, st, "d_mcur", [128, 128], BF16)
        mprev = sbt(k, st, "d_mprev", [128, 128], BF16)
        Bc = Buf("consts")
        w = sbt(k, st, "d_w", [128, 8, 768], BF16)
        Bw = Buf("w")
        hTc = [sbt(k, st, "d_hT%d" % i, [128, 8, 512], BF16) for i in range(2)]
        BhT = [Buf("a"), Buf("b")]
        qS = sbt(k, st, "d_qS", [128, 2, S], BF16)
        kS = sbt(k, st, "d_kS", [128, 2, S], BF16)
        BqS, BkS = Buf("qS"), Buf("kS")
        V = sbt(k, st, "d_V", [128, NT, 4 * 65], BF16)
        BV = Buf("V")
        vst = [sbt(k, st, "d_vst%d" % i, [128, 4, 65], BF16) for i in range(2)]
        Bvst = [Buf("a"), Buf("b")]
        xsb = [sbt(k, st, "d_xsb%d" % i, [128, 512], BF16) for i in range(2)]
        Bxsb = [Buf("a"), Buf("b")]
        t1 = sbt(k, st, "d_t1", [128, 512], F32)
        t2 = sbt(k, st, "d_t2", [128, 512], F32)
        Bt1, Bt2 = Buf("t1"), Buf("t2")
        NP = 3
        pt = [sbt(k, st, "d_pt%d" % i, [128, 256], BF16) for i in range(NP)]
        Bpt = [Buf("pt%d" % i) for i in range(NP)]
        rden = sbt(k, st, "d_rden", [65, 512], F32)
        Brden = Buf("rden")
        onesf = sbt(k, st, "d_onesf", [65, 64], F32)
        Bones = Buf("onesf")
        bcs = [sbt(k, st, "d_bcs%d" % i, [64, 512], F32) for i in range(2)]
        Bbcs = [Buf("a"), Buf("b")]
        ost = [sbt(k, st, "d_ost%d" % i, [64, 512], BF16) for i in range(2)]
        Bost = [Buf("a"), Buf("b")]
        for dst, nm in ((rC, "ropeC"), (rS, "ropeS"), (RT, "ropeRT"), (mcur, "maskcur"), (mprev, "maskprev")):
            p.dma("sp", dst[:, :], I[nm][:, :], Bc, writes=[Bc])
        mset(p, "dve", onesf[:, :], 1.0, [Bones])
        for i in range(2):
            mset(p, "pool", vst[i][:, :, 64:65], 1.0, [Bvst[i]])
        hT_v = k.hT_d.rearrange("(k p) t -> p k t", p=128)
        cps = 0
        cx = 0
        cv = 0
        cst = 0
        for g in range(3):
            d = DIL[g]
            L = S // d
            TPR = L // 128
            for sec in range(3):
                a = 256 + sec * 768 + g * 256
                p.dma("pool", w[:, :, sec * 256:(sec + 1) * 256], win[:, a:a + 256].rearrange("(k p) n -> p k n", p=128),
                      Bw, writes=[Bw])
            for c in range(8):
                hs = c % 2
                tok = slice(c * 512, (c + 1) * 512)
                p.dma("sp", hTc[hs][:, :, :], hT_v[:, :, tok], BhT[hs], reads=[k.B_hT[c]], writes=[BhT[hs]])
                for sec in range(2):
                    dstT, Bdst = (qS, BqS) if sec == 0 else (kS, BkS)
                    for blk in range(2):
                        bank = cps % 2
                        bankR = 2 + cps % 2
                        cps += 1
                        xs = cx % 2
                        cx += 1
                        col = sec * 256 + blk * 128
                        for kk in range(8):
                            mm(p, k.ps[bank][:, :], w[:, kk, col:col + 128], hTc[hs][:, kk, :], kk == 0, kk == 7,
                               [Bw, BhT[hs]], [k.Bps[bank]])
                        cpy(p, "act", xsb[xs][:, :], k.ps[bank][:, :], [k.Bps[bank]], [Bxsb[xs]])
                        mm(p, k.ps[bankR][:, :], RT[:, :], xsb[xs][:, :], True, True, [Bc, Bxsb[xs]], [k.Bps[bankR]])
                        tt(p, "pool", t1[:, :], xsb[xs][:, :], rC[:, tok], ALU.mult, [Bxsb[xs], Bc], [Bt1])
                        tt(p, "dve", t2[:, :], k.ps[bankR][:, :], rS[:, tok], ALU.mult, [k.Bps[bankR], Bc], [Bt2])
                        nm_ = 512 // d
                        dview = dstT[:, blk, :].rearrange("p (r m) -> p m r", r=d)[:, c * nm_:(c + 1) * nm_, :]
                        tt(p, "dve", dview, t1[:, :].rearrange("p (m r) -> p m r", r=d),
                           t2[:, :].rearrange("p (m r) -> p m r", r=d), ALU.add, [Bt1, Bt2], [Bdst])
                for t in range(4):
                    vs = cv % 2
                    cv += 1
                    bank = 4
                    for kk in range(8):
                        mm(p, k.ps[bank][:, 0:256], hTc[hs][:, kk, t * 128:(t + 1) * 128], w[:, kk, 512:768],
                           kk == 0, kk == 7, [BhT[hs], Bw], [k.Bps[bank]])
                    cpy(p, "act", vst[vs][:, :, 0:64], k.ps[bank][:, 0:256].rearrange("p (h d) -> p h d", d=64),
                        [k.Bps[bank]], [Bvst[vs]])
                    i = c * 4 + t
                    p.dma("sp", k.zv_d[g, i * 128:(i + 1) * 128, :], vst[vs][:, :, :].rearrange("p h d -> p (h d)"),
                          Bvst[vs], reads=[Bvst[vs]], writes=[k.B_zv])
            zv_r = k.zv_d[g].rearrange("(m r) f -> r m f", r=d)
            for r in range(d):
                p.dma("sp", V[:, r * TPR:(r + 1) * TPR, :], zv_r[r].rearrange("(t p) f -> p t f", p=128), BV,
                      reads=[k.B_zv], writes=[BV])
            for qt in range(NT):
                r, mt = divmod(qt, TPR)
                has_prev = mt != 0
                for h in range(4):
                    blk, hp = divmod(h, 2)
                    pr = slice(hp * 64, (hp + 1) * 64)
                    s_ = cst % NP
                    ob = 4 + cst % 2
                    cst += 1
                    qv = qS[pr, blk, qt * 128:(qt + 1) * 128]
                    lo = 0 if has_prev else 128
                    if has_prev:
                        mm(p, k.ps[s_][:, 0:128], kS[pr, blk, (qt - 1) * 128:qt * 128], qv, True, False,
                           [BqS, BkS], [k.Bps[s_]])
                        mm(p, k.ps[s_][:, 0:128], k.ident[:, :], mprev[:, :], False, True, [k.Bident, Bc], [k.Bps[s_]])
                    mm(p, k.ps[s_][:, 128:256], kS[pr, blk, qt * 128:(qt + 1) * 128], qv, True, False,
                       [BqS, BkS], [k.Bps[s_]])
                    mm(p, k.ps[s_][:, 128:256], k.ident[:, :], mcur[:, :], False, True, [k.Bident, Bc], [k.Bps[s_]])
                    actf(p, pt[s_][:, lo:256], k.ps[s_][:, lo:256], AF.Exp, [k.Bps[s_]], [Bpt[s_]], scale=0.125)
                    if has_prev:
                        mm(p, k.ps[ob][0:65, 0:128], V[:, qt - 1, h * 65:(h + 1) * 65], pt[s_][:, 0:128], True, False,
                           [BV, Bpt[s_]], [k.Bps[ob]])
                    mm(p, k.ps[ob][0:65, 0:128], V[:, qt, h * 65:(h + 1) * 65], pt[s_][:, 128:256], not has_prev, True,
                       [BV, Bpt[s_]], [k.Bps[ob]])
                    aview = acc[:, h, :].rearrange("p (m r) -> p r m", r=d)[:, r, mt * 128:(mt + 1) * 128]
                    if g == 0:
                        cpy(p, "dve", aview, k.ps[ob][0:65, 0:128], [k.Bps[ob]], [Bacc[h]])
                    else:
                        tt(p, "dve", aview, aview, k.ps[ob][0:65, 0:128], ALU.add, [k.Bps[ob], Bacc[h]], [Bacc[h]])
        cch = 0
        for h in range(4):
            for c in range(8):
                sl = cch % 2
                cch += 1
                tok = slice(c * 512, (c + 1) * 512)
                p.op("dve", (lambda e, h=h, tok=tok: e.reciprocal(out=rden[64:65, :], in_=acc[64:65, h, tok])),
                     [Bacc[h]], [Brden])
                mm(p, k.ps[6][0:64, :], onesf[64:65, 0:64], rden[64:65, :], True, True, [Bones, Brden], [k.Bps[6]])
                cpy(p, "act", bcs[sl][:, :], k.ps[6][0:64, :], [k.Bps[6]], [Bbcs[sl]])
                tt(p, "dve", ost[sl][:, :], acc[0:64, h, tok], bcs[sl][:, :], ALU.mult, [Bacc[h], Bbcs[sl]], [Bost[sl]])
                p.dma("sp", k.mixT_d[256 + h * 64:256 + (h + 1) * 64, tok], ost[sl][:, :], Bost[sl],
                      reads=[Bost[sl]], writes=[k.B_mix[c]])
        p.flush()


def even_mixer(k, l, s, last):
    li = l // 2
    even_pool_phase(k, li)
    even_attn_phase(k, li)
    outproj_phase(k, k.I["w_out_even"][li], 4)
    phase_epilogue(k, s, last, first=(s == k.s0))


_CACHE = {}


def _run(inputs, s0=0, s1=8, debug=()):
    key = (s0, s1, tuple(debug))
    if key not in _CACHE:
        _CACHE[key] = build(s0=s0, s1=s1, debug=debug)
    nc = _CACHE[key]
    need = needed_inputs(s0, s1)
    consts = host_consts()
    B = inputs["x"].shape[0]
    in_maps = []
    for b in range(B):
        m = {}
        for name, shape in IN_SPECS:
            if name not in need:
                continue
            a = np.asarray(inputs[name])
            if name in ("x", "c"):
                a = a[b]
            m[name] = np.ascontiguousarray(a, dtype=np.float32)
        m.update(consts)
        in_maps.append(m)
    res = run_bass_kernel_spmd(nc, in_maps, core_ids=list(range(B)))
    return res


def kernel(**inputs):
    res = _run(inputs)
    return np.stack([np.asarray(r["out"]) for r in res.results], axis=0).astype(np.float32)
```
